# Optimizing a Trainium2 kernel written in Bass

```python
import math
import jax, jax.numpy as jnp
from jax import lax
import numpy as np

D_MODEL = 1024
BATCH = 8
SEQ = 8192
DEPTH = 4

N_MEM = 256
N_MIXERS = 2
N_S5 = (DEPTH + 1) // 2
N_RET = DEPTH // 2

S5_GROUP = 16
S5_GROUPS = D_MODEL // S5_GROUP
S5_STATE = 64
DT_MIN = 1e-3
DT_MAX = 1e-1

RET_HEADS = 4
RET_QK_DIM = D_MODEL // RET_HEADS
RET_V_DIM = 2 * RET_QK_DIM
RET_VALUE_WIDTH = RET_HEADS * RET_V_DIM
RET_IN_WIDTH = 2 * D_MODEL + 2 * RET_VALUE_WIDTH
RET_CHUNK = 128
ROPE_BASE = 10000.0

XATTN_HEADS = 4
XATTN_DIM = D_MODEL // XATTN_HEADS

D_FF = 2816
MACARON_WEIGHT = 0.5
EPS = 1e-6

kernel_name = "hybrid_s5_retention_macaron_memxattn"


def _rmsnorm(x, g):
    xf = x.astype(jnp.float32)
    y = xf * lax.rsqrt(jnp.mean(xf * xf, axis=-1, keepdims=True) + EPS) * g.astype(jnp.float32)
    return y.astype(x.dtype)


def _swiglu(x, w_in, w_out):
    a, b = jnp.split(x @ w_in, 2, axis=-1)
    return (jax.nn.silu(a) * b) @ w_out


def _complex_linear_combine(e1, e2):
    a1r, a1i, b1r, b1i = e1
    a2r, a2i, b2r, b2i = e2
    ar = a2r * a1r - a2i * a1i
    ai = a2r * a1i + a2i * a1r
    br = a2r * b1r - a2i * b1i + b2r
    bi = a2r * b1i + a2i * b1r + b2i
    return ar, ai, br, bi


def _s5_mixer(u, lam_re, lam_im, log_dt, b_re, b_im, c_re, c_im, d_skip, w_glu):
    bsz, seq, _ = u.shape
    f32 = jnp.float32
    uf = u.astype(f32)
    lr = lam_re.astype(f32)
    li = lam_im.astype(f32)
    dt = jnp.exp(log_dt.astype(f32))[:, None]
    mag = jnp.exp(lr * dt)
    ar = mag * jnp.cos(li * dt)
    ai = mag * jnp.sin(li * dt)
    den = lr * lr + li * li
    nr = ar - 1.0
    zr = (nr * lr + ai * li) / den
    zi = (ai * lr - nr * li) / den
    br = b_re.astype(f32)
    bi = b_im.astype(f32)
    bbar_re = zr[..., None] * br - zi[..., None] * bi
    bbar_im = zr[..., None] * bi + zi[..., None] * br
    ug = uf.reshape(bsz, seq, S5_GROUPS, S5_GROUP)
    bu_re = jnp.einsum('bsgh,gph->sbgp', ug, bbar_re)
    bu_im = jnp.einsum('bsgh,gph->sbgp', ug, bbar_im)
    a_re = jnp.broadcast_to(ar, (seq, 1) + ar.shape)
    a_im = jnp.broadcast_to(ai, (seq, 1) + ai.shape)
    _, _, xr, xi = lax.associative_scan(_complex_linear_combine, (a_re, a_im, bu_re, bu_im), axis=0)
    y = (jnp.einsum('sbgp,ghp->bsgh', xr, c_re.astype(f32))
         - jnp.einsum('sbgp,ghp->bsgh', xi, c_im.astype(f32)))
    y = y.reshape(bsz, seq, D_MODEL) + d_skip.astype(f32) * uf
    y = jax.nn.gelu(y).astype(u.dtype)
    a, gate = jnp.split(y @ w_glu, 2, axis=-1)
    return a * jax.nn.sigmoid(gate)


def _rotary(t, cos, sin):
    t1, t2 = jnp.split(t, 2, axis=-1)
    return jnp.concatenate([t1 * cos - t2 * sin, t1 * sin + t2 * cos], axis=-1)


def _retention_mixer(u, positions, w_in, w_out):
    bsz, seq, _ = u.shape
    f32 = jnp.float32
    h = u @ w_in
    q, k, v, g = jnp.split(h, [D_MODEL, 2 * D_MODEL, 2 * D_MODEL + RET_VALUE_WIDTH], axis=-1)
    q = q.astype(f32).reshape(bsz, seq, RET_HEADS, RET_QK_DIM)
    k = k.astype(f32).reshape(bsz, seq, RET_HEADS, RET_QK_DIM) * (RET_QK_DIM ** -0.5)
    v = v.astype(f32).reshape(bsz, seq, RET_HEADS, RET_V_DIM)
    half = RET_QK_DIM // 2
    inv_freq = 1.0 / (ROPE_BASE ** jnp.linspace(0.0, 1.0, half, dtype=f32))
    ang = positions.astype(f32)[..., None] * inv_freq
    cos = jnp.cos(ang)[:, :, None, :]
    sin = jnp.sin(ang)[:, :, None, :]
    q = _rotary(q, cos, sin)
    k = _rotary(k, cos, sin)
    gamma = 1.0 - jnp.exp2(-5.0 - jnp.arange(RET_HEADS, dtype=f32))
    lg = jnp.log(gamma)
    idx = jnp.arange(RET_CHUNK)
    diff = idx[:, None] - idx[None, :]
    d_mat = jnp.where(diff >= 0, jnp.exp(lg[:, None, None] * jnp.maximum(diff, 0).astype(f32)), 0.0)
    xi = jnp.exp(lg[:, None] * (idx + 1).astype(f32))
    zeta = jnp.exp(lg[:, None] * (RET_CHUNK - 1 - idx).astype(f32))
    gamma_chunk = jnp.exp(lg * RET_CHUNK)
    n_chunks = seq // RET_CHUNK

    def to_chunks(t):
        d = t.shape[-1]
        return t.reshape(bsz, n_chunks, RET_CHUNK, RET_HEADS, d).transpose(1, 0, 3, 2, 4)

    def step(state, inp):
        qc, kc, vc = inp
        inner = jnp.einsum('bhid,bhjd->bhij', qc, kc) * d_mat
        out = (jnp.einsum('bhij,bhjv->bhiv', inner, vc)
               + jnp.einsum('bhid,bhdv->bhiv', qc, state) * xi[None, :, :, None])
        state = (state * gamma_chunk[None, :, None, None]
                 + jnp.einsum('bhjd,bhjv->bhdv', kc * zeta[None, :, :, None], vc))
        return state, out

    state0 = jnp.zeros((bsz, RET_HEADS, RET_QK_DIM, RET_V_DIM), f32)
    _, outs = lax.scan(step, state0, (to_chunks(q), to_chunks(k), to_chunks(v)))
    o = outs.transpose(1, 0, 3, 2, 4).reshape(bsz, seq, RET_HEADS, RET_V_DIM)
    mu = jnp.mean(o, axis=-1, keepdims=True)
    var = jnp.mean(jnp.square(o - mu), axis=-1, keepdims=True)
    o = ((o - mu) * lax.rsqrt(var + EPS)).reshape(bsz, seq, RET_VALUE_WIDTH)
    y = (jax.nn.silu(g.astype(f32)) * o).astype(u.dtype)
    return y @ w_out


def _memory_cross_attention(xn, mem_n, w_q, w_kv, w_o):
    bsz, seq, _ = xn.shape
    q = (xn @ w_q).reshape(bsz, seq, XATTN_HEADS, XATTN_DIM)
    k, v = jnp.split(mem_n @ w_kv, 2, axis=-1)
    k = k.reshape(bsz, N_MEM, XATTN_HEADS, XATTN_DIM)
    v = v.reshape(bsz, N_MEM, XATTN_HEADS, XATTN_DIM)
    s = jnp.einsum('bshd,bmhd->bhsm', q.astype(jnp.float32), k.astype(jnp.float32)) * (XATTN_DIM ** -0.5)
    p = jax.nn.softmax(s, axis=-1).astype(xn.dtype)
    o = jnp.einsum('bhsm,bmhd->bshd', p, v).reshape(bsz, seq, D_MODEL)
    return o @ w_o


def setup_inputs(seed: int = 0) -> dict:
    key = jax.random.key(seed)
    ks = jax.random.split(key, 32)
    f32 = jnp.float32
    nrm = lambda k, shape, scale: jax.random.normal(k, shape, f32) * scale
    x = nrm(ks[0], (BATCH, SEQ, D_MODEL), 1.0)
    mem = nrm(ks[1], (BATCH, N_MEM, D_MODEL), 1.0)
    offset = jax.random.randint(ks[2], (BATCH, 1), 0, 4096, dtype=jnp.int32)
    positions = offset + jnp.arange(SEQ, dtype=jnp.int32)[None, :]
    norm_gains = 1.0 + nrm(ks[3], (DEPTH, 4, D_MODEL), 0.02)
    mem_norm = 1.0 + nrm(ks[4], (D_MODEL,), 0.02)
    final_norm = 1.0 + nrm(ks[5], (D_MODEL,), 0.02)
    ffn1_w_in = nrm(ks[6], (DEPTH, D_MODEL, 2 * D_FF), D_MODEL ** -0.5)
    ffn1_w_out = nrm(ks[7], (DEPTH, D_FF, D_MODEL), D_FF ** -0.5)
    ffn2_w_in = nrm(ks[8], (DEPTH, D_MODEL, 2 * D_FF), D_MODEL ** -0.5)
    ffn2_w_out = nrm(ks[9], (DEPTH, D_FF, D_MODEL), D_FF ** -0.5)
    n_idx = jnp.arange(S5_STATE, dtype=f32)
    s5_lam_re = -0.5 + nrm(ks[10], (N_S5, S5_GROUPS, S5_STATE), 0.01)
    s5_lam_im = math.pi * n_idx[None, None, :] + nrm(ks[11], (N_S5, S5_GROUPS, S5_STATE), 0.01)
    s5_log_dt = jax.random.uniform(ks[12], (N_S5, S5_GROUPS), f32, math.log(DT_MIN), math.log(DT_MAX))
    s5_b_re = nrm(ks[13], (N_S5, S5_GROUPS, S5_STATE, S5_GROUP), (2 * S5_GROUP) ** -0.5)
    s5_b_im = nrm(ks[14], (N_S5, S5_GROUPS, S5_STATE, S5_GROUP), (2 * S5_GROUP) ** -0.5)
    s5_c_re = nrm(ks[15], (N_S5, S5_GROUPS, S5_GROUP, S5_STATE), S5_STATE ** -0.5)
    s5_c_im = nrm(ks[16], (N_S5, S5_GROUPS, S5_GROUP, S5_STATE), S5_STATE ** -0.5)
    s5_d = nrm(ks[17], (N_S5, D_MODEL), 1.0)
    s5_w_glu = nrm(ks[18], (N_S5, D_MODEL, 2 * D_MODEL), D_MODEL ** -0.5)
    ret_w_in = nrm(ks[19], (N_RET, D_MODEL, RET_IN_WIDTH), D_MODEL ** -0.5)
    ret_w_out = nrm(ks[20], (N_RET, RET_VALUE_WIDTH, D_MODEL), RET_VALUE_WIDTH ** -0.5)
    xattn_w_q = nrm(ks[21], (DEPTH, D_MODEL, D_MODEL), D_MODEL ** -0.5)
    xattn_w_kv = nrm(ks[22], (DEPTH, D_MODEL, 2 * D_MODEL), D_MODEL ** -0.5)
    xattn_w_o = nrm(ks[23], (DEPTH, D_MODEL, D_MODEL), D_MODEL ** -0.5)
    return {
        "x": x, "mem": mem, "positions": positions,
        "norm_gains": norm_gains, "mem_norm": mem_norm, "final_norm": final_norm,
        "ffn1_w_in": ffn1_w_in, "ffn1_w_out": ffn1_w_out,
        "ffn2_w_in": ffn2_w_in, "ffn2_w_out": ffn2_w_out,
        "s5_lam_re": s5_lam_re, "s5_lam_im": s5_lam_im, "s5_log_dt": s5_log_dt,
        "s5_b_re": s5_b_re, "s5_b_im": s5_b_im, "s5_c_re": s5_c_re, "s5_c_im": s5_c_im,
        "s5_d": s5_d, "s5_w_glu": s5_w_glu,
        "ret_w_in": ret_w_in, "ret_w_out": ret_w_out,
        "xattn_w_q": xattn_w_q, "xattn_w_kv": xattn_w_kv, "xattn_w_o": xattn_w_o,
    }


def reference(x, mem, positions, norm_gains, mem_norm, final_norm,
              ffn1_w_in, ffn1_w_out, ffn2_w_in, ffn2_w_out,
              s5_lam_re, s5_lam_im, s5_log_dt, s5_b_re, s5_b_im, s5_c_re, s5_c_im, s5_d, s5_w_glu,
              ret_w_in, ret_w_out, xattn_w_q, xattn_w_kv, xattn_w_o):
    mem_n = _rmsnorm(mem, mem_norm)
    for i in range(DEPTH):
        g = norm_gains[i]
        x = x + MACARON_WEIGHT * _swiglu(_rmsnorm(x, g[0]), ffn1_w_in[i], ffn1_w_out[i])
        h = _rmsnorm(x, g[1])
        j = i // N_MIXERS
        if i % N_MIXERS == 0:
            mix = _s5_mixer(h, s5_lam_re[j], s5_lam_im[j], s5_log_dt[j], s5_b_re[j], s5_b_im[j],
                            s5_c_re[j], s5_c_im[j], s5_d[j], s5_w_glu[j])
        else:
            mix = _retention_mixer(h, positions, ret_w_in[j], ret_w_out[j])
        x = x + mix
        x = x + _memory_cross_attention(_rmsnorm(x, g[2]), mem_n, xattn_w_q[i], xattn_w_kv[i], xattn_w_o[i])
        x = x + MACARON_WEIGHT * _swiglu(_rmsnorm(x, g[3]), ffn2_w_in[i], ffn2_w_out[i])
    return _rmsnorm(x, final_norm)
```

```python
import math
import os
from contextlib import ExitStack

import numpy as np
import concourse.bass as bass
import concourse.mybir as mybir
from concourse.bass_utils import run_bass_kernel_spmd

F32 = mybir.dt.float32
BF16 = mybir.dt.bfloat16
I32 = mybir.dt.int32
ALU = mybir.AluOpType
AF = mybir.ActivationFunctionType

D = 1024
NCH = 8
DFF = 2816
HC = 22
NMEM = 256
EPS = 1e-6
T = 512
BLK = 2048
NRING = 11
PAGE = 256
TWO_PI = 2.0 * math.pi


class Op:
    __slots__ = ("eng", "fn", "deps", "inc", "tok", "is_dma", "idx")

    def __init__(self, eng, fn, is_dma):
        self.eng = eng
        self.fn = fn
        self.deps = []
        self.inc = False
        self.tok = None
        self.is_dma = is_dma


class V:
    __slots__ = ("ap", "keys")

    def __init__(self, ap, keys):
        self.ap = ap
        self.keys = keys

    def w(self, ap):
        return V(ap, self.keys)


class Buf:
    def __init__(self, space, ap, off, shape, esize):
        self.space = space
        self.ap = ap
        self.off = off
        self.shape = tuple(shape)
        self.esize = esize
        st = []
        s = esize
        for n in reversed(self.shape):
            st.append(s)
            s *= n
        self.strides = tuple(reversed(st))

    def __getitem__(self, idx):
        if not isinstance(idx, tuple):
            idx = (idx,)
        idx = idx + (slice(None),) * (len(self.shape) - len(idx))
        rngs = []
        for i, n in zip(idx, self.shape):
            if isinstance(i, int):
                rngs.append((i, i + 1))
            else:
                a, b, _ = i.indices(n)
                rngs.append((a, b))
        ap = self.ap[(slice(None),) + idx]
        keys = set()
        inner = rngs[-1]
        offs = [self.off]
        for (a, b), st in zip(rngs[:-1], self.strides[:-1]):
            offs = [o + k * st for o in offs for k in range(a, b)]
        lo_in = inner[0] * self.strides[-1]
        hi_in = inner[1] * self.strides[-1]
        for o in offs:
            for pg in range((o + lo_in) // PAGE, (o + hi_in - 1) // PAGE + 1):
                keys.add((self.space, pg))
        return V(ap, frozenset(keys))

    def all(self):
        return self[(slice(None),) * len(self.shape)]


class Sched:
    def __init__(self):
        self.streams = {e: [] for e in ("pe", "act", "dve", "pool", "sp")}
        self.last_w = {}
        self.readers = {}
        self.dreaders = {}

    def add(self, eng, fn, reads=(), writes=(), dma=False):
        op = Op(eng, fn, dma)
        rk = set()
        for v in reads:
            rk |= v.keys if isinstance(v, V) else {v}
        wk = set()
        for v in writes:
            wk |= v.keys if isinstance(v, V) else {v}
        deps = {}
        for k in rk:
            w = self.last_w.get(k)
            if w is not None:
                deps[id(w)] = (w, True)
        for k in wk:
            w = self.last_w.get(k)
            if w is not None and id(w) not in deps:
                deps[id(w)] = (w, False)
            r = self.readers.get(k)
            if r:
                for o in r.values():
                    if id(o) not in deps:
                        deps[id(o)] = (o, False)
            dr = self.dreaders.get(k)
            if dr:
                for o in dr:
                    if id(o) not in deps:
                        deps[id(o)] = (o, False)
        for d, raw in deps.values():
            if (not dma) and (not d.is_dma) and d.eng == eng and not raw:
                continue
            op.deps.append(d)
            d.inc = True
        for k in rk:
            if k in wk:
                continue
            if dma:
                self.dreaders.setdefault(k, []).append(op)
            else:
                self.readers.setdefault(k, {})[eng] = op
        for k in wk:
            self.last_w[k] = op
            if k in self.readers:
                self.readers[k] = {}
            if k in self.dreaders:
                self.dreaders[k] = []
        self.streams[eng].append(op)
        return op

    def emit(self, nc, es):
        NDS = {"sp": 28, "pool": 20, "act": 12}
        esem = {e: es.enter_context(nc.semaphore("c_" + e)) for e in ("pe", "act", "dve", "pool")}
        dsem = {q: [es.enter_context(nc.semaphore("d_%s%d" % (q, i))) for i in range(n)] for q, n in NDS.items()}
        for e, ops in self.streams.items():
            cnt = 0
            nd = 0
            hist = []
            for op in ops:
                if op.is_dma:
                    n = NDS[e]
                    op.tok = (dsem[e][nd % n], 16 * (nd // n + 1))
                    if nd >= n:
                        op.deps.append(hist[nd - n])
                    hist.append(op)
                    nd += 1
                elif op.inc:
                    cnt += 1
                    op.tok = (esem[e], cnt)
        block = es.enter_context(nc.Block())
        streams = self.streams

        def body(e):
            def run(eng):
                waited = {}
                for op in streams[e]:
                    for d in op.deps:
                        s, v = d.tok
                        if waited.get(id(s), 0) < v:
                            eng.wait_ge(s, v)
                            waited[id(s)] = v
                    ins = op.fn(eng)
                    if op.is_dma:
                        ins.then_inc(op.tok[0], 16)
                    elif op.inc:
                        ins.then_inc(op.tok[0], 1)
            return run

        block.sync(body("sp"))
        block.tensor(body("pe"))
        block.scalar(body("act"))
        block.vector(body("dve"))
        block.gpsimd(body("pool"))


class Prog:
    def __init__(self, seq, depth, parts=("ffn1", "mix", "xattn", "ffn2"), final=True):
        self.seq = seq
        self.nt = seq // T
        self.depth = depth
        self.parts = parts
        self.final = final
        self.S = Sched()
        self.nc = bass.Bass("TRN2", target_bir_lowering=False)
        self.es = ExitStack()
        self.dram = {}
        self.blocks = []
        self.blkid = {}
        self.ring_ctr = 0
        self.ps_ctr = 0
        self.sb_off = 0
        self.alt = 0

    def din(self, name, shape, dt=F32):
        t = self.nc.dram_tensor(name, list(shape), dt, kind="ExternalInput").ap()
        self.dram[name] = t
        return t

    def dv(self, ap, key):
        return V(ap, frozenset([key]))

    def sb(self, shape, dt=F32):
        es = {F32: 4, BF16: 2, I32: 4}[dt]
        n = int(np.prod(shape))
        nbytes = (n * es + 255) // 256 * 256
        off = self.sb_off
        self.sb_off += nbytes
        return ("sb", off, tuple(shape), dt, es, n)

    def carve(self, base_off, shape, dt):
        es = {F32: 4, BF16: 2, I32: 4}[dt]
        n = int(np.prod(shape))
        return ("sb", base_off, tuple(shape), dt, es, n)

    def realize(self, spec):
        _, off, shape, dt, es, n = spec
        ap = self.SB[:, off // 4: off // 4 + (n * es + 3) // 4]
        if dt != F32:
            ap = ap.bitcast(dt)
            ap = ap[:, 0:n]
        if len(shape) == 2:
            ap = ap.rearrange("p (a b) -> p a b", a=shape[0])
        elif len(shape) == 3:
            ap = ap.rearrange("p (a b c) -> p a b c", a=shape[0], b=shape[1])
        return Buf("sb", ap, off, shape, es)

    def mm(self, out, lhsT, rhs, start, stop):
        self.S.add("pe", lambda e: e.matmul(out.ap, lhsT=lhsT.ap, rhs=rhs.ap, start=start, stop=stop),
                   reads=[lhsT, rhs], writes=[out])

    def tr(self, out, in_, ident):
        self.S.add("pe", lambda e: e.transpose(out=out.ap, in_=in_.ap, identity=ident.ap),
                   reads=[in_, ident], writes=[out])

    def act(self, out, in_, func, bias=None, scale=None, eng="act"):
        kw = {}
        rd = [in_]
        if bias is not None:
            if isinstance(bias, V):
                kw["bias"] = bias.ap
                rd.append(bias)
            else:
                kw["bias"] = bias
        if scale is not None:
            if isinstance(scale, V):
                kw["scale"] = scale.ap
                rd.append(scale)
            else:
                kw["scale"] = scale
        self.S.add("act", lambda e: e.activation(out=out.ap, in_=in_.ap, func=func, **kw), reads=rd, writes=[out])

    def veng(self):
        self.alt ^= 1
        return "dve" if self.alt else "pool"

    def tt(self, eng, out, a, b, op):
        self.S.add(eng, lambda e: e.tensor_tensor(out=out.ap, in0=a.ap, in1=b.ap, op=op), reads=[a, b], writes=[out])

    def ts(self, eng, out, a, s1, op0, s2=None, op1=None):
        rd = [a]
        s1a = s1
        s2a = s2
        if isinstance(s1, V):
            rd.append(s1)
            s1a = s1.ap
        if isinstance(s2, V):
            rd.append(s2)
            s2a = s2.ap
        if op1 is None:
            self.S.add(eng, lambda e: e.tensor_scalar(out=out.ap, in0=a.ap, scalar1=s1a, scalar2=None, op0=op0),
                       reads=rd, writes=[out])
        else:
            self.S.add(eng, lambda e: e.tensor_scalar(out=out.ap, in0=a.ap, scalar1=s1a, scalar2=s2a, op0=op0, op1=op1),
                       reads=rd, writes=[out])

    def stt(self, eng, out, a, s, b, op0, op1):
        rd = [a, b]
        sa = s
        if isinstance(s, V):
            rd.append(s)
            sa = s.ap
        self.S.add(eng, lambda e: e.scalar_tensor_tensor(out=out.ap, in0=a.ap, scalar=sa, in1=b.ap, op0=op0, op1=op1),
                   reads=rd, writes=[out])

    def cp(self, eng, out, in_):
        if eng == "act":
            self.S.add("act", lambda e: e.activation(out=out.ap, in_=in_.ap, func=AF.Copy), reads=[in_], writes=[out])
        else:
            self.S.add(eng, lambda e: e.tensor_copy(out=out.ap, in_=in_.ap), reads=[in_], writes=[out])

    def memset(self, eng, out, val):
        self.S.add(eng, lambda e: e.memset(out.ap, val), writes=[out])

    def dma(self, q, out, in_):
        self.S.add(q, lambda e: e.dma_start(out=out.ap, in_=in_.ap), reads=[in_], writes=[out], dma=True)

    def psb(self):
        b = self.ps_ctr % 8
        self.ps_ctr += 1
        return b

    def add_block(self, name, pieces, n):
        self.blkid[name] = len(self.blocks)
        self.blocks.append(dict(pieces=pieces, n=n, name=name))

    def wload(self, name, shape):
        bid = self.blkid[name]
        n = self.blocks[bid]["n"]
        slot = self.ring_ctr % NRING
        self.ring_ctr += 1
        dst = self.ring[slot, 0:n]
        self.dma("sp", dst, self.dv(self.wscr[bid, :, 0:n], ("wscr", bid)))
        off = self.ring.off + slot * BLK * 2
        ap = self.ring.ap[:, slot, 0:n].rearrange("p (a b) -> p a b", a=shape[0])
        return Buf("sb", ap, off, shape, 2)

    def build(self):
        nc = self.nc
        S = self.S
        seq, depth = self.seq, self.depth
        n_s5 = (depth + 1) // 2
        n_ret = depth // 2
        if "mix" not in self.parts:
            n_s5 = n_ret = 0
        self.n_s5, self.n_ret = n_s5, n_ret
        x = self.din("x", [seq, D])
        mem = self.din("mem", [NMEM, D])
        pos = self.din("pos", [1, seq], I32)
        gains_d = self.din("gains", [128, 4 * depth + 2, NCH])
        cst_d = self.din("cst", [128, 1408])
        ffn_in = [[self.din("ffn%d_in_%d" % (w, l), [D, 2 * DFF]) for l in range(depth)] for w in (1, 2)]
        ffn_out = [[self.din("ffn%d_out_%d" % (w, l), [DFF, D]) for l in range(depth)] for w in (1, 2)]
        xq = [self.din("xq_%d" % l, [D, D]) for l in range(depth)]
        xkv = [self.din("xkv_%d" % l, [D, 2 * D]) for l in range(depth)]
        xo = [self.din("xo_%d" % l, [D, D]) for l in range(depth)]
        glu = [self.din("glu_%d" % j, [D, 2 * D]) for j in range(n_s5)]
        s5m = [self.din("s5m_%d" % j, [4, 32, 128, 128]) for j in range(n_s5)]
        s5p = [self.din("s5p_%d" % j, [128, 3, 32]) for j in range(n_s5)]
        s5d = self.din("s5d", [128, n_s5, NCH]) if n_s5 else None
        rin = [self.din("rin_%d" % j, [D, 6 * D]) for j in range(n_ret)]
        rout = [self.din("rout_%d" % j, [2 * D, D]) for j in range(n_ret)]
        out = nc.dram_tensor("out", [seq, D], F32, kind="ExternalOutput").ap()

        def wpiece(w, r0, nk, c0, ncol):
            ap = w[r0:r0 + nk * 128, c0:c0 + ncol].rearrange("(k p) n -> p k n", p=128)
            return ap

        for l in range(depth):
            for wi in range(2):
                for j in range(HC):
                    self.add_block(("fi", wi, l, j), [(wpiece(ffn_in[wi][l], 0, 8, j * 128, 128), 0, 256, 128),
                                                      (wpiece(ffn_in[wi][l], 0, 8, DFF + j * 128, 128), 128, 256, 128)], 2048)
                for oc in range(8):
                    self.add_block(("fo", wi, l, oc, 0), [(wpiece(ffn_out[wi][l], 0, 16, oc * 128, 128), 0, 128, 128)], 2048)
                    self.add_block(("fo", wi, l, oc, 1), [(wpiece(ffn_out[wi][l], 2048, 6, oc * 128, 128), 0, 128, 128)], 768)
            for j in range(4):
                self.add_block(("xq", l, j), [(wpiece(xq[l], 0, 8, j * 256, 256), 0, 256, 256)], 2048)
                self.add_block(("xo", l, j), [(wpiece(xo[l], 0, 8, j * 256, 256), 0, 256, 256)], 2048)
            self.add_block(("xk", l), None, 2048)
            self.add_block(("xv", l), None, 2048)
            if "mix" not in self.parts:
                continue
            if l % 2 == 0:
                j5 = l // 2
                for j in range(8):
                    self.add_block(("glu", j5, j), [(wpiece(glu[j5], 0, 8, j * 128, 128), 0, 256, 128),
                                                    (wpiece(glu[j5], 0, 8, D + j * 128, 128), 128, 256, 128)], 2048)
                for m in range(4):
                    for h in range(2):
                        ap = s5m[j5][m, h * 16:(h + 1) * 16].rearrange("g p n -> p g n")
                        self.add_block(("s5m", j5, m, h), [(ap, 0, 128, 128)], 2048)
                for h in range(2):
                    for tname in ("wre", "wim", "cos", "sin"):
                        self.add_block(("s5t", j5, tname, h), None, 2048)
            else:
                jr = l // 2
                for j in range(8):
                    self.add_block(("rqk", jr, j), [(wpiece(rin[jr], 0, 8, j * 256, 256), 0, 256, 256)], 2048)
                for j in range(8):
                    for kh in range(2):
                        self.add_block(("rvg", jr, j, kh), [(wpiece(rin[jr], kh * 512, 4, 2 * D + j * 512, 512), 0, 512, 512)], 2048)
                for oc in range(8):
                    self.add_block(("ro", jr, oc), [(wpiece(rout[jr], 0, 16, oc * 128, 128), 0, 128, 128)], 2048)
        nblk = len(self.blocks)
        self.wscr = nc.dram_tensor("wscr", [nblk, 128, BLK], BF16, kind="Internal").ap()
        rst_d = nc.dram_tensor("rstate", [max(n_ret, 1), 128, 8 * 512], F32, kind="Internal").ap()
        self.s5d0 = nc.dram_tensor("s5d0", [max(n_s5, 1), 2, 128, 2048], F32, kind="Internal").ap()
        self.psf = None

        sp = {}
        sp["xT"] = self.sb([NCH, T])
        sp["xn"] = self.sb([NCH, T], BF16)
        sp["ring"] = self.sb([NRING, BLK], BF16)
        sp["rst"] = self.sb([8, 512])
        sp["rstb"] = self.sb([8, 512], BF16)
        sp["io"] = self.sb([2, D])
        sp["gains"] = self.sb([4 * depth + 2, NCH])
        sp["cst"] = self.sb([1408])
        sp["identb"] = self.sb([128], BF16)
        sp["onesm"] = self.sb([128], BF16)
        sp["ones1"] = self.sb([128], BF16)
        sp["rstd"] = self.sb([T])
        sp["lnb"] = self.sb([T])
        sp["sq"] = self.sb([2, T], BF16)
        sp["cos"] = self.sb([T])
        sp["sin"] = self.sb([T])
        sp["s5d"] = self.sb([max(n_s5, 1), NCH])
        sp["s5c"] = self.sb([max(n_s5, 1), 8, 32])
        sp["small"] = self.sb([64])
        sp["s5dd"] = self.sb([max(n_s5, 1), NCH, 128], BF16)
        U0 = self.sb_off
        USZ = 80 * 1024
        self.sb_off += USZ
        total = self.sb_off
        assert total <= 212800, total
        self.SB = self.es.enter_context(nc.sbuf_tensor("SB", [128, total // 4], F32))
        PS = self.es.enter_context(nc.psum_tensor("PS", [128, 8 * 512], F32))
        R = {k: self.realize(v) for k, v in sp.items()}
        self.ring = R["ring"]
        xT, xn = R["xT"], R["xn"]
        psf = Buf("ps", PS[:].rearrange("p (a b) -> p a b", a=8), 0, (8, 512), 4)
        psb16 = Buf("ps", PS[:].bitcast(BF16).rearrange("p (a b) -> p a b", a=8), 0, (8, 1024), 2)
        gains, cst = R["gains"], R["cst"]
        ident = Buf("sb", cst.ap[:, 0:128], cst.off, (128,), 4)
        identb, onesm, ones1 = R["identb"], R["onesm"], R["ones1"]
        rstd, lnb, sq = R["rstd"], R["lnb"], R["sq"]

        def U(off, shape, dt=F32):
            b = self.realize(self.carve(U0 + off, shape, dt))
            assert off + int(np.prod(shape)) * b.esize <= USZ, (off, shape)
            return b

        self.dma("pool", gains.all(), self.dv(gains_d[:, :, :], "gains_d"))
        self.dma("pool", cst.all(), self.dv(cst_d[:, :], "cst_d"))
        if n_s5:
            self.dma("pool", R["s5d"].all(), self.dv(s5d[:, :, :], "s5d_d"))
        self.cp("dve", identb.all(), ident.all())
        for j5 in range(n_s5):
            for c in range(NCH):
                self.ts("dve", R["s5dd"][j5, c], ident.all(), R["s5d"][j5, c:c + 1], ALU.mult)
        self.memset("dve", onesm.all(), 1.0 / D)
        self.memset("dve", ones1.all(), 1.0)

        def rmsnorm(src, gi, dst, N):
            pb = self.psb()
            for c in range(NCH):
                s_ = sq[c % 2, 0:N]
                self.act(s_, src[c, 0:N], AF.Square)
                self.mm(psf[pb, 0:N], onesm.all(), s_, c == 0, c == NCH - 1)
            self.act(lnb[0:N], psf[pb, 0:N], AF.Ln, bias=EPS)
            self.act(rstd[0:N], lnb[0:N], AF.Exp, scale=-0.5)
            for c in range(NCH):
                self.stt("dve", dst[c, 0:N], src[c, 0:N], gains[gi, c:c + 1], rstd[0:N], ALU.mult, ALU.mult)

        def range_reduce(eng, a, tmpf, tmpi):
            self.ts(eng, tmpf, a, 1.0 / TWO_PI, ALU.mult)
            self.cp(eng, tmpi, tmpf)
            self.cp(eng, tmpf, tmpi)
            self.stt(eng, a, tmpf, -TWO_PI, a, ALU.mult, ALU.add)
            self.ts(eng, tmpf, a, math.pi, ALU.is_gt, -TWO_PI, ALU.mult)
            self.tt(eng, a, a, tmpf, ALU.add)
            self.ts(eng, tmpf, a, -math.pi, ALU.is_lt, TWO_PI, ALU.mult)
            self.tt(eng, a, a, tmpf, ALU.add)

        stg_f = [U(i * 8192, [BLK]) for i in range(3)]
        stg_b = [U(24576 + i * 4096, [BLK], BF16) for i in range(3)]
        ci = 0
        cengs = ["dve", "pool", "act"]
        for bid, b in enumerate(self.blocks):
            if b["pieces"] is None:
                continue
            sf, sbf = stg_f[ci % 3], stg_b[ci % 3]
            n = b["n"]
            for (ap, doff, rowlen, ncol) in b["pieces"]:
                nk = ap.shape[1]
                dstap = sf.ap[:, 0:nk * rowlen].rearrange("p (k n) -> p k n", k=nk)[:, :, doff:doff + ncol]
                dst = V(dstap, sf[0:nk * rowlen].keys)
                self.dma("sp", dst, self.dv(ap, ("w", b["name"][0])))
            self.cp(cengs[ci % 3], sbf[0:n], sf[0:n])
            self.dma("pool", self.dv(self.wscr[bid, :, 0:n], ("wscr", bid)), sbf[0:n])
            ci += 1

        memT = U(36864, [NCH, NMEM])
        memn = U(36864 + 8192, [NCH, NMEM], BF16)
        mstage = R["io"]
        for mc in range(2):
            self.dma("sp", mstage[mc], self.dv(mem[mc * 128:(mc + 1) * 128, :], "mem_d"))
            for half in range(2):
                pb = self.psb()
                for c4 in range(4):
                    c = half * 4 + c4
                    self.tr(psf[pb, c4 * 128:(c4 + 1) * 128], mstage[mc, c * 128:(c + 1) * 128], ident.all())
                o = memT[half * 4:(half + 1) * 4, mc * 128:(mc + 1) * 128]
                self.cp("dve", o, V(psf.ap[:, pb, :].rearrange("p (a b) -> p a b", a=4), psf[pb].keys))
        rmsnorm(memT, 4 * depth, memn, NMEM)
        kst = U(36864 + 12288, [NCH, NMEM], BF16)
        vst = U(36864 + 16384, [2, D], BF16)
        for l in range(depth):
            for half in range(2):
                for pn in range(4):
                    sf, sbf = stg_f[ci % 3], stg_b[ci % 3]
                    ci += 1
                    c0 = half * D + pn * 256
                    dstap = sf.ap[:, 0:2048].rearrange("p (k n) -> p k n", k=8)
                    self.dma("sp", V(dstap, sf.all().keys), self.dv(wpiece(xkv[l], 0, 8, c0, 256), ("w", "xkv")))
                    self.cp(cengs[ci % 3], sbf.all(), sf.all())
                    wv = Buf("sb", sbf.ap.rearrange("p (k n) -> p k n", k=8), sbf.off, (8, 256), 2)
                    if half == 0:
                        for j in range(2):
                            oc = pn * 2 + j
                            pb = self.psb()
                            for kc in range(8):
                                self.mm(psf[pb, 0:NMEM], wv[kc, j * 128:(j + 1) * 128], memn[kc], kc == 0, kc == 7)
                            self.cp("act", kst[oc], psf[pb, 0:NMEM])
                    else:
                        for mc in range(2):
                            pb = self.psb()
                            for kc in range(8):
                                self.mm(psf[pb, 0:256], memn[kc, mc * 128:(mc + 1) * 128], wv[kc], kc == 0, kc == 7)
                            self.cp("act", vst[mc, pn * 256:(pn + 1) * 256], psf[pb, 0:256])
            bk, bv = self.blkid[("xk", l)], self.blkid[("xv", l)]
            self.dma("pool", self.dv(self.wscr[bk, :, :], ("wscr", bk)), V(kst.ap.rearrange("p a b -> p (a b)"), kst.all().keys))
            self.dma("pool", self.dv(self.wscr[bv, :, :], ("wscr", bv)), V(vst.ap.rearrange("p a b -> p (a b)"), vst.all().keys))

        s5c = R["s5c"]
        for j5 in range(n_s5):
            self.s5_prologue(j5, s5p[j5], s5c, U, cst, range_reduce)

        cosb, sinb = R["cos"], R["sin"]
        io = R["io"]
        for t in range(self.nt):
            tok0 = t * T
            for sub in range(4):
                st = io[sub % 2]
                self.dma("pool", st, self.dv(x[tok0 + sub * 128: tok0 + (sub + 1) * 128, :], "x_d"))
                for half in range(2):
                    pb = self.psb()
                    for c4 in range(4):
                        c = half * 4 + c4
                        self.tr(psf[pb, c4 * 128:(c4 + 1) * 128], io[sub % 2, c * 128:(c + 1) * 128], ident.all())
                    o = xT[half * 4:(half + 1) * 4, sub * 128:(sub + 1) * 128]
                    self.cp("dve" if half else "act", o, V(psf.ap[:, pb, :].rearrange("p (a b) -> p a b", a=4), psf[pb].keys))
            if n_ret:
                self.rotary_tables(pos, tok0, cst, cosb, sinb, U, range_reduce)
            for l in range(depth):
                if "ffn1" in self.parts:
                    self.ffn(l, 0, 4 * l + 0, rmsnorm, psf, xT, xn, U)
                if "mix" in self.parts:
                    if l % 2 == 0:
                        self.s5_layer(l // 2, 4 * l + 1, rmsnorm, psf, xT, xn, U, R, t)
                    else:
                        self.ret_layer(l // 2, 4 * l + 1, rmsnorm, psf, psb16, xT, xn, U, R, t, rst_d, cst, identb)
                if "xattn" in self.parts:
                    self.xattn(l, 4 * l + 2, rmsnorm, psf, xT, xn, U, ones1)
                if "ffn2" in self.parts:
                    self.ffn(l, 1, 4 * l + 3, rmsnorm, psf, xT, xn, U)
            if self.final:
                rmsnorm(xT, 4 * depth + 1, xT, T)
            for sub in range(4):
                st = io[sub % 2]
                for half in range(2):
                    pb = self.psb()
                    for c4 in range(4):
                        c = half * 4 + c4
                        self.tr(psf[pb, c4 * 128:(c4 + 1) * 128], xT[c, sub * 128:(sub + 1) * 128], ident.all())
                    self.cp("dve" if half else "act", io[sub % 2, half * 512:(half + 1) * 512], psf[pb])
                self.dma("pool", self.dv(out[tok0 + sub * 128: tok0 + (sub + 1) * 128, :], ("out", t, sub)), st)
        outs = [op for op in S.streams["pool"] if op.is_dma]
        last = S.add("sp", lambda e: e.nop(), reads=[("out", t_, s_) for t_ in range(self.nt) for s_ in range(4)])
        S.emit(nc, self.es)
        self.es.close()
        return nc

    def ffn(self, l, wi, gi, rmsnorm, psf, xT, xn, U):
        rmsnorm(xT, gi, xn, T)
        g = U(0, [HC, T], BF16)
        sa = U(22528, [2, T])
        for j in range(HC):
            w = self.wload(("fi", wi, l, j), (8, 256))
            pa, pb = self.psb(), self.psb()
            for kc in range(8):
                self.mm(psf[pa], w[kc, 0:128], xn[kc], kc == 0, kc == 7)
            for kc in range(8):
                self.mm(psf[pb], w[kc, 128:256], xn[kc], kc == 0, kc == 7)
            self.act(sa[j % 2], psf[pa], AF.Silu)
            self.tt("dve", g[j], sa[j % 2], psf[pb], ALU.mult)
        for oc in range(8):
            w1 = self.wload(("fo", wi, l, oc, 0), (16, 128))
            w2 = self.wload(("fo", wi, l, oc, 1), (6, 128))
            po = self.psb()
            for kc in range(HC):
                wv = w1[kc] if kc < 16 else w2[kc - 16]
                self.mm(psf[po], wv, g[kc], kc == 0, kc == HC - 1)
            self.stt("dve", xT[oc], psf[po], 0.5, xT[oc], ALU.mult, ALU.add)

    def xattn(self, l, gi, rmsnorm, psf, xT, xn, U, ones1):
        rmsnorm(xT, gi, xn, T)
        qT = U(0, [NCH, T], BF16)
        oT = U(8192, [NCH, T], BF16)
        ex = U(16384, [2, T], BF16)
        rden = U(18432, [T])
        for j in range(4):
            w = self.wload(("xq", l, j), (8, 256))
            for i in range(2):
                oc = j * 2 + i
                pb = self.psb()
                for kc in range(8):
                    self.mm(psf[pb], w[kc, i * 128:(i + 1) * 128], xn[kc], kc == 0, kc == 7)
                self.cp("act", qT[oc], psf[pb])
        kT = self.wload(("xk", l), (NCH, NMEM))
        vv = self.wload(("xv", l), (2, D))
        for h in range(4):
            for mc in range(2):
                pb = self.psb()
                for dc in range(2):
                    self.mm(psf[pb], kT[2 * h + dc, mc * 128:(mc + 1) * 128], qT[2 * h + dc], dc == 0, dc == 1)
                self.act(ex[mc], psf[pb], AF.Exp, scale=1.0 / 16.0)
            pd = self.psb()
            for mc in range(2):
                self.mm(psf[pd], ones1.all(), ex[mc], mc == 0, mc == 1)
            self.S.add("dve", lambda e, o=rden.all(), i=psf[pd]: e.reciprocal(out=o.ap, in_=i.ap), reads=[psf[pd]], writes=[rden.all()])
            for dc in range(2):
                po = self.psb()
                for mc in range(2):
                    self.mm(psf[po], vv[mc, h * 256 + dc * 128: h * 256 + (dc + 1) * 128], ex[mc], mc == 0, mc == 1)
                self.tt("dve", oT[2 * h + dc], psf[po], rden.all(), ALU.mult)
        for j in range(4):
            w = self.wload(("xo", l, j), (8, 256))
            for i in range(2):
                oc = j * 2 + i
                pb = self.psb()
                for kc in range(8):
                    self.mm(psf[pb], w[kc, i * 128:(i + 1) * 128], oT[kc], kc == 0, kc == 7)
                self.tt("dve", xT[oc], psf[pb], xT[oc], ALU.add)

    def rotary_tables(self, pos, tok0, cst, cosb, sinb, U, range_reduce):
        ang = U(61440, [T])
        ang2 = U(63488, [T])
        tmpf = U(65536, [T])
        tmpi = U(67584, [T], I32)
        posi = U(69632, [T], I32)
        self.dma("pool", posi.all(), self.dv(pos[0:1, tok0:tok0 + T].partition_broadcast(128), "pos_d"))
        self.cp("dve", ang.all(), posi.all())
        self.ts("dve", ang.all(), ang.all(), cst[1156:1157], ALU.mult)
        self.ts("dve", ang2.all(), ang.all(), math.pi / 2, ALU.add)
        range_reduce("dve", ang.all(), tmpf.all(), tmpi.all())
        range_reduce("dve", ang2.all(), tmpf.all(), tmpi.all())
        self.act(sinb.all(), ang.all(), AF.Sin)
        self.act(cosb.all(), ang2.all(), AF.Sin)

    def ret_layer(self, jr, gi, rmsnorm, psf, psb16, xT, xn, U, R, t, rst_d, cst, identb):
        rst, rstb = R["rst"], R["rstb"]
        cosb, sinb = R["cos"], R["sin"]
        rmsnorm(xT, gi, xn, T)
        qk = U(0, [16, T], BF16)
        Vt = U(16384, [4, 2048], BF16)
        Gt = U(32768, [4, 2048], BF16)
        yT = U(49152, [16, T], BF16)
        mt = U(65536, [4, T])
        PT = U(73728, [2, 128], BF16)
        kz = U(74240, [2, 256], BF16)
        qxi = U(75264, [2, 2, 128], BF16)
        stats = U(76288, [2, 8])
        on = U(76544, [T])
        ybuf = U(78592, [2, T], BF16)
        dmaskT = Buf("sb", cst.ap[:, 128:640].rearrange("p (h i) -> p h i", h=4), cst.off + 128 * 4, (4, 128), 4)
        xi = Buf("sb", cst.ap[:, 640:1152].rearrange("p (h i) -> p h i", h=4), cst.off + 640 * 4, (4, 128), 4)
        if t == 0:
            self.memset("pool", rst.all(), 0.0)
            self.memset("pool", rstb.all(), 0.0)
        else:
            self.dma("pool", V(rst.ap.rearrange("p a b -> p (a b)"), rst.all().keys), self.dv(rst_d[jr, :, :], ("rst_d", jr)))
            for i in range(8):
                self.cp("pool" if i % 2 else "act", rstb[i], rst[i])
        for j in range(8):
            w = self.wload(("rqk", jr, j), (8, 256))
            p1, p2 = self.psb(), self.psb()
            for kc in range(8):
                self.mm(psf[p1], w[kc, 0:128], xn[kc], kc == 0, kc == 7)
            for kc in range(8):
                self.mm(psf[p2], w[kc, 128:256], xn[kc], kc == 0, kc == 7)
            self.tt("dve", mt[0], psf[p1], cosb.all(), ALU.mult)
            self.tt("dve", mt[1], psf[p2], sinb.all(), ALU.mult)
            self.tt("dve", mt[2], psf[p1], sinb.all(), ALU.mult)
            self.tt("dve", mt[3], psf[p2], cosb.all(), ALU.mult)
            self.tt("pool", qk[2 * j], mt[0], mt[1], ALU.subtract)
            self.tt("pool", qk[2 * j + 1], mt[2], mt[3], ALU.add)
        for j in range(8):
            w0 = self.wload(("rvg", jr, j, 0), (4, 512))
            w1 = self.wload(("rvg", jr, j, 1), (4, 512))
            for ch in range(4):
                pb = self.psb()
                for kc in range(8):
                    wv = w0[kc] if kc < 4 else w1[kc - 4]
                    self.mm(psf[pb], xn[kc, ch * 128:(ch + 1) * 128], wv, kc == 0, kc == 7)
                if j < 4:
                    self.cp("act" if ch % 2 else "dve", Vt[ch, j * 512:(j + 1) * 512], psf[pb])
                else:
                    self.act(Gt[ch, (j - 4) * 512:(j - 3) * 512], psf[pb], AF.Silu)
        for ch in range(4):
            tk = slice(ch * 128, (ch + 1) * 128)
            for h in range(4):
                r2 = (ch * 4 + h) % 2
                vh = Vt[ch, h * 512:(h + 1) * 512]
                pbk = self.psb()
                for dc in range(2):
                    self.tr(psb16[pbk, dc * 128:(dc + 1) * 128], qk[8 + 2 * h + dc, tk], identb.all())
                self.ts("dve", kz[r2], psb16[pbk, 0:256], cst[1152 + h:1153 + h], ALU.mult)
                pbi = self.psb()
                for dc in range(2):
                    self.mm(psf[pbi, 0:128], qk[8 + 2 * h + dc, tk], qk[2 * h + dc, tk], dc == 0, dc == 1)
                self.tt("dve", PT[r2], psf[pbi, 0:128], dmaskT[h], ALU.mult)
                for dc in range(2):
                    self.tt("pool", qxi[r2, dc], qk[2 * h + dc, tk], xi[h], ALU.mult)
                pbo = self.psb()
                self.mm(psf[pbo], PT[r2], vh, True, False)
                for dc in range(2):
                    self.mm(psf[pbo], qxi[r2, dc], rstb[2 * h + dc], False, dc == 1)
                for dc in range(2):
                    pbs = self.psb()
                    self.mm(psf[pbs], kz[r2, dc * 128:(dc + 1) * 128], vh, True, True)
                    self.stt("dve", rst[2 * h + dc], rst[2 * h + dc], cst[1157 + h:1158 + h], psf[pbs], ALU.mult, ALU.add)
                    self.cp("act", rstb[2 * h + dc], rst[2 * h + dc])
                st6 = stats[r2, 0:6]
                mv = stats[r2, 6:8]
                self.S.add("dve", lambda e, o=st6, i=psf[pbo]: e.bn_stats(out=o.ap, in_=i.ap), reads=[psf[pbo]], writes=[st6])
                self.S.add("dve", lambda e, o=mv, i=st6: e.bn_aggr(out=o.ap, in_=i.ap), reads=[st6], writes=[mv])
                self.act(stats[r2, 7:8], stats[r2, 7:8], AF.Ln, bias=EPS)
                self.act(stats[r2, 7:8], stats[r2, 7:8], AF.Exp, scale=-0.5)
                self.ts("dve", on.all(), psf[pbo], stats[r2, 6:7], ALU.subtract, stats[r2, 7:8], ALU.mult)
                self.tt("pool", ybuf[r2], on.all(), Gt[ch, h * 512:(h + 1) * 512], ALU.mult)
                pby = self.psb()
                for vc in range(4):
                    self.tr(psb16[pby, vc * 128:(vc + 1) * 128], ybuf[r2, vc * 128:(vc + 1) * 128], identb.all())
                src = V(psb16.ap[:, pby, 0:512].rearrange("p (a b) -> p a b", a=4), psb16[pby, 0:512].keys)
                self.cp("act", yT[h * 4:(h + 1) * 4, tk], src)
        for oc in range(8):
            w = self.wload(("ro", jr, oc), (16, 128))
            pb = self.psb()
            for kc in range(16):
                self.mm(psf[pb], w[kc], yT[kc], kc == 0, kc == 15)
            self.tt("dve", xT[oc], psf[pb], xT[oc], ALU.add)
        if t < self.nt - 1:
            self.dma("pool", self.dv(rst_d[jr, :, :], ("rst_d", jr)), V(rst.ap.rearrange("p a b -> p (a b)"), rst.all().keys))

    def s5_prologue(self, j5, s5p, s5c, U, cst, range_reduce):
        if int(os.environ.get("KDBG_S5", "99")) < 0:
            return
        P = U(0, [3, 32])
        W = U(512, [12, 32])
        tmpf = U(2048, [32])
        tmpi = U(2304, [32], I32)
        self.dma("sp", P.all(), self.dv(s5p[:, :, :], ("s5p", j5)))
        lr, li, ldt = P[0], P[1], P[2]
        dt, lrdt, sn, cs, ar, ai, nr, den, a_, b_, sr, cr = [W[i] for i in range(12)]
        r, rr_re, rr_im, th, zr, zi = s5c[j5, 0], s5c[j5, 1], s5c[j5, 2], s5c[j5, 5], s5c[j5, 6], s5c[j5, 7]
        th1 = lambda gb: s5c[j5, 5, gb:gb + 1]
        zr1 = lambda gb: s5c[j5, 6, gb:gb + 1]
        zi1 = lambda gb: s5c[j5, 7, gb:gb + 1]
        r1 = lambda gb: s5c[j5, 0, gb:gb + 1]
        E = "dve"
        self.act(dt, ldt, AF.Exp)
        self.tt(E, lrdt, lr, dt, ALU.mult)
        self.tt(E, th, li, dt, ALU.mult)
        self.act(r, lrdt, AF.Exp)
        self.cp(E, a_, th)
        self.ts(E, b_, th, math.pi / 2, ALU.add)
        range_reduce(E, a_, tmpf.all(), tmpi.all())
        range_reduce(E, b_, tmpf.all(), tmpi.all())
        self.act(sn, a_, AF.Sin)
        self.act(cs, b_, AF.Sin)
        self.tt(E, ar, r, cs, ALU.mult)
        self.tt(E, ai, r, sn, ALU.mult)
        self.ts(E, nr, ar, -1.0, ALU.add)
        self.tt(E, den, lr, lr, ALU.mult)
        self.tt(E, a_, li, li, ALU.mult)
        self.tt(E, den, den, a_, ALU.add)
        self.S.add(E, lambda e, o=den: e.reciprocal(out=o.ap, in_=o.ap), reads=[den], writes=[den])
        self.tt(E, a_, nr, lr, ALU.mult)
        self.tt(E, b_, ai, li, ALU.mult)
        self.tt(E, a_, a_, b_, ALU.add)
        self.tt(E, zr, a_, den, ALU.mult)
        self.tt(E, a_, ai, lr, ALU.mult)
        self.tt(E, b_, nr, li, ALU.mult)
        self.tt(E, a_, a_, b_, ALU.subtract)
        self.tt(E, zi, a_, den, ALU.mult)
        self.ts(E, a_, th, 128.0, ALU.mult)
        self.ts(E, b_, a_, math.pi / 2, ALU.add)
        range_reduce(E, a_, tmpf.all(), tmpi.all())
        range_reduce(E, b_, tmpf.all(), tmpi.all())
        self.act(sr, a_, AF.Sin)
        self.act(cr, b_, AF.Sin)
        self.tt(E, rr_re, r, cr, ALU.mult)
        self.tt(E, rr_im, r, sr, ALU.mult)
        iota = Buf("sb", cst.ap[:, 1280:1408], cst.off + 1280 * 4, (128,), 4)
        ang = U(4096, [16, 128])
        ang2 = U(12288, [16, 128])
        tf = U(20480, [16, 128])
        ti = U(28672, [16, 128], I32)
        d0 = U(36864, [16, 128])
        tb = [U(45056 + i * 4096, [16, 128], BF16) for i in range(4)]
        for h in range(2):
            for g in range(16):
                gb = h * 16 + g
                self.ts(E, ang[g], iota.all(), th1(gb), ALU.mult)
            self.ts(E, ang2.all(), ang.all(), math.pi / 2, ALU.add)
            range_reduce(E, ang.all(), tf.all(), ti.all())
            range_reduce(E, ang2.all(), tf.all(), ti.all())
            self.act(ang.all(), ang.all(), AF.Sin)
            self.act(ang2.all(), ang2.all(), AF.Sin)
            self.cp("pool", tb[2].all(), ang2.all())
            self.cp("pool", tb[3].all(), ang.all())
            self.memset("pool", d0.all(), 1.0)
            for g in range(16):
                gb = h * 16 + g
                self.ts(E, tf[g], ang2[g], zr1(gb), ALU.mult)
                self.stt(E, tb[0][g], ang[g], zi1(gb), tf[g], ALU.mult, ALU.add)
                self.ts(E, tf[g], ang[g], zr1(gb), ALU.mult)
                self.stt(E, tb[1][g], ang2[g], zi1(gb), tf[g], ALU.mult, ALU.subtract)
                self.ts("pool", d0[g], d0[g], r1(gb), ALU.mult)
            self.memset("pool", d0[:, 0:1], 0.0)
            for i, nm in enumerate(("wre", "wim", "cos", "sin")):
                bid = self.blkid[("s5t", j5, nm, h)]
                self.dma("pool", self.dv(self.wscr[bid, :, :], ("wscr", bid)), V(tb[i].ap.rearrange("p a b -> p (a b)"), tb[i].all().keys))
            self.dma("pool", self.dv(self.s5d0[j5, h, :, :], ("s5d0", j5, h)), V(d0.ap.rearrange("p a b -> p (a b)"), d0.all().keys))

    def s5_layer(self, j5, gi, rmsnorm, psf, xT, xn, U, R, t):
        DBG = int(os.environ.get("KDBG_S5", "99"))
        if DBG < 1:
            return
        s5c, s5d, small, s5dd = R["s5c"], R["s5d"], R["small"], R["s5dd"]
        rmsnorm(xT, gi, xn, T)
        yT = U(0, [NCH, T], BF16)
        tabs = [U(8192 + i * 4096, [16, 128], BF16) for i in range(4)]
        wre, wim, cosT, sinT = tabs
        d0 = U(24576, [16, 128])
        bur = U(32768, [16, 128])
        bui = U(40960, [16, 128])
        T1 = U(49152, [16, 128])
        T2 = U(57344, [16, 128])
        X1 = U(65536, [16, 128], BF16)
        X2 = U(69632, [16, 128], BF16)
        ytmp = U(73728, [4, 128])
        sg = U(75776, [T])
        tg = U(77824, [T])
        if t == 0:
            self.memset("pool", s5c[j5, 3], 0.0)
            self.memset("pool", s5c[j5, 4], 0.0)
        for half in range(2):
            hs = slice(half * 16, (half + 1) * 16)
            for i, nm in enumerate(("wre", "wim", "cos", "sin")):
                bid = self.blkid[("s5t", j5, nm, half)]
                self.dma("sp", V(tabs[i].ap.rearrange("p a b -> p (a b)"), tabs[i].all().keys), self.dv(self.wscr[bid, :, :], ("wscr", bid)))
            self.dma("sp", V(d0.ap.rearrange("p a b -> p (a b)"), d0.all().keys), self.dv(self.s5d0[j5, half, :, :], ("s5d0", j5, half)))
            Bre = self.wload(("s5m", j5, 0, half), (16, 128))
            Bim = self.wload(("s5m", j5, 1, half), (16, 128))
            Cre = self.wload(("s5m", j5, 2, half), (16, 128))
            Cim = self.wload(("s5m", j5, 3, half), (16, 128))
            cre_, cim_ = s5c[j5, 3, hs], s5c[j5, 4, hs]
            rre, rim = s5c[j5, 1, hs], s5c[j5, 2, hs]
            for sc in range(4):
                tk = slice(sc * 128, (sc + 1) * 128)
                for q in range(4):
                    pr, pi_ = self.psb(), self.psb()
                    for g4 in range(4):
                        g = q * 4 + g4
                        gb = half * 16 + g
                        self.mm(psf[pr, g4 * 128:(g4 + 1) * 128], Bre[g], xn[gb // 4, tk], True, True)
                    for g4 in range(4):
                        g = q * 4 + g4
                        gb = half * 16 + g
                        self.mm(psf[pi_, g4 * 128:(g4 + 1) * 128], Bim[g], xn[gb // 4, tk], True, True)
                    self.cp("act", V(bur.ap[:, q * 4:(q + 1) * 4, :].rearrange("p a b -> p (a b)"), bur[q * 4:(q + 1) * 4].keys), psf[pr])
                    self.cp("act", V(bui.ap[:, q * 4:(q + 1) * 4, :].rearrange("p a b -> p (a b)"), bui[q * 4:(q + 1) * 4].keys), psf[pi_])
                if DBG < 2:
                    continue
                self.tt("dve", T1.all(), bur.all(), wre.all(), ALU.mult)
                self.tt("pool", T2.all(), bur.all(), wim.all(), ALU.mult)
                self.tt("dve", bur.all(), bui.all(), wim.all(), ALU.mult)
                self.tt("pool", bui.all(), bui.all(), wre.all(), ALU.mult)
                self.tt("dve", T1.all(), T1.all(), bur.all(), ALU.subtract)
                self.tt("pool", T2.all(), T2.all(), bui.all(), ALU.add)
                if DBG < 3:
                    continue
                c3 = lambda v: V(v.ap.unsqueeze(2), v.keys)
                self.tt("dve", T1[:, 0:1], T1[:, 0:1], c3(cre_), ALU.add)
                self.tt("pool", T2[:, 0:1], T2[:, 0:1], c3(cim_), ALU.add)
                if DBG < 4:
                    continue
                for TT in (T1, T2):
                    self.S.add("dve", lambda e, o=TT.all(), d=d0.all(): e.tensor_tensor_scan(out=o.ap.rearrange("p a b -> p (a b)"), data0=d.ap.rearrange("p a b -> p (a b)"), data1=o.ap.rearrange("p a b -> p (a b)"), initial=0.0, op0=ALU.mult, op1=ALU.add),
                               reads=[TT.all(), d0.all()], writes=[TT.all()])
                if DBG < 5:
                    continue
                zlr = V(T1.ap[:, :, 127], T1[:, 127:128].keys)
                zli = V(T2.ap[:, :, 127], T2[:, 127:128].keys)
                a_, b_, a2, b2 = small[0:16], small[16:32], small[32:48], small[48:64]
                self.tt("dve", a_, rre, zlr, ALU.mult)
                self.tt("dve", b_, rim, zli, ALU.mult)
                self.tt("dve", a2, rre, zli, ALU.mult)
                self.tt("dve", b2, rim, zlr, ALU.mult)
                self.tt("dve", cre_, a_, b_, ALU.subtract)
                self.tt("dve", cim_, a2, b2, ALU.add)
                if DBG < 6:
                    continue
                self.tt("dve", bur.all(), T1.all(), cosT.all(), ALU.mult)
                self.tt("pool", bui.all(), T2.all(), sinT.all(), ALU.mult)
                self.tt("dve", X1.all(), bur.all(), bui.all(), ALU.subtract)
                self.tt("dve", bur.all(), T1.all(), sinT.all(), ALU.mult)
                self.tt("pool", bui.all(), T2.all(), cosT.all(), ALU.mult)
                self.stt("dve", X2.all(), bur.all(), -1.0, bui.all(), ALU.mult, ALU.subtract)
                if DBG < 7:
                    continue
                pc = self.psb()
                for c4 in range(4):
                    for gq in range(4):
                        g = c4 * 4 + gq
                        self.mm(psf[pc, c4 * 128:(c4 + 1) * 128], Cre[g], X1[g], gq == 0, False)
                        self.mm(psf[pc, c4 * 128:(c4 + 1) * 128], Cim[g], X2[g], False, False)
                    self.mm(psf[pc, c4 * 128:(c4 + 1) * 128], s5dd[j5, half * 4 + c4], xn[half * 4 + c4, tk], False, True)
                src = V(psf.ap[:, pc, :].rearrange("p (a b) -> p a b", a=4), psf[pc].keys)
                self.act(yT[half * 4:(half + 1) * 4, tk], src, AF.Gelu_apprx_tanh)
        if DBG < 8:
            return
        for j in range(8):
            w = self.wload(("glu", j5, j), (8, 256))
            pa, pg = self.psb(), self.psb()
            for kc in range(8):
                self.mm(psf[pa], w[kc, 0:128], yT[kc], kc == 0, kc == 7)
            for kc in range(8):
                self.mm(psf[pg], w[kc, 128:256], yT[kc], kc == 0, kc == 7)
            self.act(sg.all(), psf[pg], AF.Sigmoid)
            self.tt("dve", tg.all(), psf[pa], sg.all(), ALU.mult)
            self.tt("pool", xT[j], xT[j], tg.all(), ALU.add)


def host_consts():
    cst = np.zeros((128, 1408), np.float64)
    cst[:, 0:128] = np.eye(128)
    lg = np.log(1.0 - np.exp2(-5.0 - np.arange(4)))
    idx = np.arange(128)
    diff = idx[None, :] - idx[:, None]
    for h in range(4):
        cst[:, 128 + h * 128:128 + (h + 1) * 128] = np.where(diff >= 0, np.exp(lg[h] * np.maximum(diff, 0)), 0.0) / 16.0
        cst[:, 640 + h * 128:640 + (h + 1) * 128] = np.exp(lg[h] * (idx + 1))[None, :]
        cst[:, 1152 + h] = np.exp(lg[h] * (127 - idx)) / 16.0
        cst[:, 1157 + h] = np.exp(lg[h] * 128)
    cst[:, 1156] = 1.0 / (10000.0 ** np.linspace(0.0, 1.0, 128))
    cst[:, 1280:1408] = idx[None, :]
    return cst.astype(np.float32)


def prep_inputs(inp, b, seq, depth, mix=True):
    n_s5 = (depth + 1) // 2
    n_ret = depth // 2
    m = {}
    m["x"] = np.ascontiguousarray(inp["x"][b, :seq])
    m["mem"] = np.ascontiguousarray(inp["mem"][b])
    m["pos"] = np.ascontiguousarray(inp["positions"][b:b + 1, :seq]).astype(np.int32)
    g = np.concatenate([inp["norm_gains"][:depth].reshape(depth * 4, D), inp["mem_norm"][None], inp["final_norm"][None]], 0)
    m["gains"] = np.ascontiguousarray(g.reshape(-1, NCH, 128).transpose(2, 0, 1))
    m["cst"] = host_consts()
    for l in range(depth):
        m["ffn1_in_%d" % l] = inp["ffn1_w_in"][l]
        m["ffn2_in_%d" % l] = inp["ffn2_w_in"][l]
        m["ffn1_out_%d" % l] = inp["ffn1_w_out"][l]
        m["ffn2_out_%d" % l] = inp["ffn2_w_out"][l]
        m["xq_%d" % l] = inp["xattn_w_q"][l]
        m["xkv_%d" % l] = inp["xattn_w_kv"][l]
        m["xo_%d" % l] = inp["xattn_w_o"][l]
    if not mix:
        return m
    for j in range(n_s5):
        m["glu_%d" % j] = inp["s5_w_glu"][j]
        mats = np.zeros((4, 32, 128, 128), np.float32)
        for gb in range(32):
            for two in range(2):
                g = 2 * gb + two
                r0 = (g % 8) * 16
                mats[0, gb, r0:r0 + 16, two * 64:(two + 1) * 64] = inp["s5_b_re"][j, g].T
                mats[1, gb, r0:r0 + 16, two * 64:(two + 1) * 64] = inp["s5_b_im"][j, g].T
                mats[2, gb, two * 64:(two + 1) * 64, r0:r0 + 16] = inp["s5_c_re"][j, g].T
                mats[3, gb, two * 64:(two + 1) * 64, r0:r0 + 16] = inp["s5_c_im"][j, g].T
        m["s5m_%d" % j] = mats
        p = np.zeros((128, 3, 32), np.float32)
        for gb in range(32):
            for two in range(2):
                g = 2 * gb + two
                p[two * 64:(two + 1) * 64, 0, gb] = inp["s5_lam_re"][j, g]
                p[two * 64:(two + 1) * 64, 1, gb] = inp["s5_lam_im"][j, g]
                p[two * 64:(two + 1) * 64, 2, gb] = inp["s5_log_dt"][j, g]
        m["s5p_%d" % j] = p
    if n_s5:
        m["s5d"] = np.ascontiguousarray(inp["s5_d"][:n_s5].reshape(n_s5, NCH, 128).transpose(2, 0, 1))
    for j in range(n_ret):
        m["rin_%d" % j] = inp["ret_w_in"][j]
        m["rout_%d" % j] = inp["ret_w_out"][j]
    return m


_PROG_CACHE = {}


def kernel(**inputs):
    seq = inputs["x"].shape[1]
    depth = inputs["norm_gains"].shape[0]
    nb = inputs["x"].shape[0]
    key = (seq, depth)
    if key not in _PROG_CACHE:
        _PROG_CACHE[key] = Prog(seq, depth).build()
    nc = _PROG_CACHE[key]
    inp = {k: np.asarray(v) for k, v in inputs.items()}
    in_maps = [prep_inputs(inp, b, seq, depth) for b in range(nb)]
    res = run_bass_kernel_spmd(nc, in_maps, core_ids=list(range(nb)))
    return np.stack([np.asarray(r["out"]) for r in res.results], 0).astype(np.float32)
```
